# Optimizing a Trainium2 kernel written in Bass

```python
import jax, jax.numpy as jnp
from jax import lax
import numpy as np

D_MODEL = 1024
BATCH = 32
SEQ = 2048
DEPTH = 1

MLSTM_HEADS = 4
MLSTM_HEAD_DIM = D_MODEL // 8
MLSTM_WIDTH = MLSTM_HEADS * MLSTM_HEAD_DIM
HGRN_HEADS = 4
HGRN_HEAD_DIM = D_MODEL // 8
HGRN_WIDTH = HGRN_HEADS * HGRN_HEAD_DIM
MIX_WIDTH = MLSTM_WIDTH + HGRN_WIDTH
CONV_WIDTH = 4
MLSTM_CHUNK = 64
HGRN_CHUNK = 16
D_FF = ((8 * D_MODEL // 3 + 255) // 256) * 256
FFN_RESIDUAL_WEIGHT = 0.5
EPS = 1e-6
N_MOD = 9
M_NEG = -1e30
PROJ_SPLITS = [MLSTM_WIDTH, MLSTM_WIDTH, MLSTM_WIDTH, MLSTM_WIDTH, 2 * MLSTM_HEADS,
               HGRN_WIDTH, HGRN_WIDTH, HGRN_WIDTH, HGRN_WIDTH]
PROJ_WIDTH = sum(PROJ_SPLITS)
PROJ_IDX = list(np.cumsum(PROJ_SPLITS)[:-1])

kernel_name = "hymba_mlstm_hgrn2_macaron_layer"


def rmsnorm(x, g):
    xf = x.astype(jnp.float32)
    y = xf * lax.rsqrt(jnp.mean(xf * xf, axis=-1, keepdims=True) + EPS)
    return (y * g.astype(jnp.float32)).astype(x.dtype)


def head_rmsnorm(h, g, n_heads):
    B, S, W = h.shape
    hf = h.astype(jnp.float32).reshape(B, S, n_heads, W // n_heads)
    hf = hf * lax.rsqrt(jnp.mean(hf * hf, axis=-1, keepdims=True) + EPS)
    return hf.reshape(B, S, W) * g.astype(jnp.float32)


def split_heads(a, n_heads):
    B, S, W = a.shape
    return a.reshape(B, S, n_heads, W // n_heads).transpose(0, 2, 1, 3)


def merge_heads(a):
    B, H, S, dh = a.shape
    return a.transpose(0, 2, 1, 3).reshape(B, S, H * dh)


def to_chunks(a, L):
    B, H, S = a.shape[:3]
    return jnp.moveaxis(a.reshape(B, H, S // L, L, *a.shape[3:]), 2, 0)


def from_chunks(a):
    nc, B, H, L, d = a.shape
    return jnp.moveaxis(a, 0, 2).reshape(B, H, nc * L, d)


def causal_dwconv(x, w, b):
    C = x.shape[-1]
    y = lax.conv_general_dilated(x, w.astype(x.dtype)[:, None, :], window_strides=(1,),
                                 padding=[(CONV_WIDTH - 1, 0)],
                                 dimension_numbers=('NWC', 'WIO', 'NWC'),
                                 feature_group_count=C)
    return y + b.astype(x.dtype)


def mlstm_chunkwise(q, k, v, log_i, log_f):
    B, H, S, dh = q.shape
    L = MLSTM_CHUNK
    causal = jnp.tril(jnp.ones((L, L), dtype=bool))
    xs = (to_chunks(q, L), to_chunks(k, L), to_chunks(v, L), to_chunks(log_i, L), to_chunks(log_f, L))

    def step(carry, inp):
        C, n, m = carry
        qb, kb, vb, li, lf = inp
        bcum = jnp.cumsum(lf, axis=-1)
        d = bcum[..., :, None] - bcum[..., None, :] + li[..., None, :]
        d = jnp.where(causal, d, -jnp.inf)
        inter = bcum + m[..., None]
        m_t = jnp.maximum(inter, jnp.max(d, axis=-1))
        w = jnp.exp(d - m_t[..., None])
        a = jnp.exp(inter - m_t)
        s = jnp.einsum('bhtd,bhsd->bhts', qb, kb) * w
        num = a[..., None] * jnp.einsum('bhtd,bhde->bhte', qb, C) + jnp.einsum('bhts,bhse->bhte', s, vb)
        den = a * jnp.einsum('bhtd,bhd->bht', qb, n) + jnp.sum(s, axis=-1)
        h = num / jnp.maximum(jnp.abs(den), jnp.exp(-m_t))[..., None]
        m_new = m_t[..., -1]
        g = jnp.exp(bcum[..., -1:] - bcum + li - m_new[..., None])
        decay = jnp.exp(bcum[..., -1] + m - m_new)
        C_new = decay[..., None, None] * C + jnp.einsum('bhs,bhsd,bhse->bhde', g, kb, vb)
        n_new = decay[..., None] * n + jnp.einsum('bhs,bhsd->bhd', g, kb)
        return (C_new, n_new, m_new), h

    init = (jnp.zeros((B, H, dh, dh), jnp.float32), jnp.zeros((B, H, dh), jnp.float32),
            jnp.full((B, H), M_NEG, jnp.float32))
    _, hs = lax.scan(step, init, xs)
    return from_chunks(hs)


def hgrn2_chunkwise(q, k, v, log_f):
    B, H, S, dk = q.shape
    dv = v.shape[-1]
    L = HGRN_CHUNK
    causal = jnp.tril(jnp.ones((L, L), dtype=bool))[:, :, None]
    xs = (to_chunks(q, L), to_chunks(k, L), to_chunks(v, L), to_chunks(log_f, L))

    def step(Sst, inp):
        qb, kb, vb, gb = inp
        bc = jnp.cumsum(gb, axis=2)
        inter = jnp.einsum('bhtc,bhce->bhte', qb * jnp.exp(bc), Sst)
        dec = jnp.exp(jnp.where(causal, bc[:, :, :, None, :] - bc[:, :, None, :, :], -jnp.inf))
        att = jnp.einsum('bhtc,bhsc,bhtsc->bhts', qb, kb, dec)
        o = inter + jnp.einsum('bhts,bhse->bhte', att, vb)
        S_new = jnp.exp(bc[:, :, -1])[..., None] * Sst + jnp.einsum(
            'bhsc,bhse->bhce', kb * jnp.exp(bc[:, :, -1:] - bc), vb)
        return S_new, o

    _, os = lax.scan(step, jnp.zeros((B, H, dk, dv), jnp.float32), xs)
    return from_chunks(os)


def swiglu(y, w_gate, w_up, w_down):
    return (jax.nn.silu(y @ w_gate) * (y @ w_up)) @ w_down


def token_mix(y, w_in, conv_w, conv_b, gate_b, mnorm_g, lb, hnorm_g, w_out):
    f32 = jnp.float32
    p = y @ w_in
    mq, mk, mv, mo, mif, hq, hf, hi, hg = jnp.split(p, PROJ_IDX, axis=-1)

    qk = jax.nn.silu(causal_dwconv(jnp.concatenate([mq, mk], axis=-1), conv_w, conv_b))
    mq, mk = jnp.split(qk, 2, axis=-1)
    q = split_heads(mq, MLSTM_HEADS).astype(f32)
    k = split_heads(mk, MLSTM_HEADS).astype(f32) * (MLSTM_HEAD_DIM ** -0.5)
    v = split_heads(mv, MLSTM_HEADS).astype(f32)
    gates = (mif.astype(f32) + gate_b.astype(f32)).transpose(0, 2, 1)
    log_i = gates[:, :MLSTM_HEADS]
    log_f = jax.nn.log_sigmoid(gates[:, MLSTM_HEADS:])
    hm = merge_heads(mlstm_chunkwise(q, k, v, log_i, log_f))
    out_m = head_rmsnorm(hm, mnorm_g, MLSTM_HEADS) * jax.nn.sigmoid(mo.astype(f32))

    fgate = lb.astype(f32) + (1.0 - lb.astype(f32)) * jax.nn.sigmoid(hf.astype(f32))
    hq_ = split_heads(jax.nn.silu(hq.astype(f32)), HGRN_HEADS)
    hk_ = split_heads(1.0 - fgate, HGRN_HEADS)
    hv_ = split_heads(hi.astype(f32), HGRN_HEADS)
    hlf = split_heads(jnp.log(fgate), HGRN_HEADS)
    ho = merge_heads(hgrn2_chunkwise(hq_, hk_, hv_, hlf))
    out_h = head_rmsnorm(ho, hnorm_g, HGRN_HEADS) * jax.nn.silu(hg.astype(f32))

    merged = jnp.concatenate([out_m, out_h], axis=-1).astype(y.dtype)
    return merged @ w_out


def setup_inputs(seed: int = 0) -> dict:
    key = jax.random.key(seed)
    ks = jax.random.split(key, 32)
    nrm = jax.random.normal
    D, F = D_MODEL, D_FF

    def gain(k, n):
        return 1.0 + 0.05 * nrm(k, (DEPTH, n), jnp.float32)

    forget_bias = jnp.linspace(3.0, 6.0, MLSTM_HEADS, dtype=jnp.float32)[None, :] \
        + 0.1 * nrm(ks[14], (DEPTH, MLSTM_HEADS), jnp.float32)
    input_bias = 0.1 * nrm(ks[15], (DEPTH, MLSTM_HEADS), jnp.float32)
    return {
        "x": nrm(ks[0], (BATCH, SEQ, D), jnp.float32),
        "c": nrm(ks[1], (BATCH, D), jnp.float32),
        "ada_w": 0.5 * (D ** -0.5) * nrm(ks[2], (DEPTH, D, N_MOD * D), jnp.float32),
        "ada_b": 0.02 * nrm(ks[3], (DEPTH, N_MOD * D), jnp.float32),
        "ffn1_pre_g": gain(ks[4], D),
        "ffn1_post_g": gain(ks[5], D),
        "ffn1_w_gate": (D ** -0.5) * nrm(ks[6], (DEPTH, D, F), jnp.float32),
        "ffn1_w_up": (D ** -0.5) * nrm(ks[7], (DEPTH, D, F), jnp.float32),
        "ffn1_w_down": (F ** -0.5) * nrm(ks[8], (DEPTH, F, D), jnp.float32),
        "mix_pre_g": gain(ks[9], D),
        "mix_post_g": gain(ks[10], D),
        "w_in": (D ** -0.5) * nrm(ks[11], (DEPTH, D, PROJ_WIDTH), jnp.float32),
        "mlstm_conv_w": (CONV_WIDTH ** -0.5) * nrm(ks[12], (DEPTH, CONV_WIDTH, 2 * MLSTM_WIDTH), jnp.float32),
        "mlstm_conv_b": 0.02 * nrm(ks[13], (DEPTH, 2 * MLSTM_WIDTH), jnp.float32),
        "mlstm_gate_b": jnp.concatenate([input_bias, forget_bias], axis=-1),
        "mlstm_norm_g": gain(ks[16], MLSTM_WIDTH),
        "hgrn_lb_logits": nrm(ks[17], (DEPTH + 1, HGRN_WIDTH), jnp.float32),
        "hgrn_norm_g": gain(ks[18], HGRN_WIDTH),
        "w_out": (MIX_WIDTH ** -0.5) * nrm(ks[19], (DEPTH, MIX_WIDTH, D), jnp.float32),
        "ffn2_pre_g": gain(ks[20], D),
        "ffn2_post_g": gain(ks[21], D),
        "ffn2_w_gate": (D ** -0.5) * nrm(ks[22], (DEPTH, D, F), jnp.float32),
        "ffn2_w_up": (D ** -0.5) * nrm(ks[23], (DEPTH, D, F), jnp.float32),
        "ffn2_w_down": (F ** -0.5) * nrm(ks[24], (DEPTH, F, D), jnp.float32),
    }


def reference(x, c, ada_w, ada_b, ffn1_pre_g, ffn1_post_g, ffn1_w_gate, ffn1_w_up, ffn1_w_down,
              mix_pre_g, mix_post_g, w_in, mlstm_conv_w, mlstm_conv_b, mlstm_gate_b, mlstm_norm_g,
              hgrn_lb_logits, hgrn_norm_g, w_out, ffn2_pre_g, ffn2_post_g, ffn2_w_gate, ffn2_w_up,
              ffn2_w_down):
    lb_all = jnp.cumsum(jax.nn.softmax(hgrn_lb_logits.astype(jnp.float32), axis=0), axis=0)
    sc = jax.nn.silu(c)
    h = x
    for l in range(DEPTH):
        mod = (sc @ ada_w[l] + ada_b[l])[:, None, :]
        (sh1, sc1, g1, sh2, sc2, g2, sh3, sc3, g3) = jnp.split(mod, N_MOD, axis=-1)

        y = rmsnorm(h, ffn1_pre_g[l]) * (1.0 + sc1) + sh1
        y = swiglu(y, ffn1_w_gate[l], ffn1_w_up[l], ffn1_w_down[l])
        h = h + FFN_RESIDUAL_WEIGHT * g1 * rmsnorm(y, ffn1_post_g[l])

        y = rmsnorm(h, mix_pre_g[l]) * (1.0 + sc2) + sh2
        y = token_mix(y, w_in[l], mlstm_conv_w[l], mlstm_conv_b[l], mlstm_gate_b[l], mlstm_norm_g[l],
                      lb_all[l], hgrn_norm_g[l], w_out[l])
        h = h + g2 * rmsnorm(y, mix_post_g[l])

        y = rmsnorm(h, ffn2_pre_g[l]) * (1.0 + sc3) + sh3
        y = swiglu(y, ffn2_w_gate[l], ffn2_w_up[l], ffn2_w_down[l])
        h = h + FFN_RESIDUAL_WEIGHT * g3 * rmsnorm(y, ffn2_post_g[l])
    return h
```

```python
import math
from contextlib import ExitStack
import numpy as np
import concourse.bass as bass
import concourse.mybir as mybir
from concourse.bass_utils import run_bass_kernel_spmd

F32 = mybir.dt.float32
BF16 = mybir.dt.bfloat16
AF = mybir.ActivationFunctionType
ALU = mybir.AluOpType

D = 1024
DFF = 2816
NFC = 22
TT = 512
EPS = 1e-6
NCORES = 8
import os
GROUP_CAST = os.environ.get("K_GC", "1") == "1"
GROUP_C = os.environ.get("K_GS", "1") == "1"
NSLOT = 5
SLOTLEN = 4096
LNSCALE = math.log(128.0 ** -0.5)
SQD = math.sqrt(float(D))

PV_PRE = (0, 32, 64)
PV_POST = (8, 40, 72)
PV_ADAB = 80
PV_CW = 152
PV_CB = 184
PV_N = 192


class Op:
    __slots__ = ("eng", "fn", "deps", "sig", "cnt", "epoch", "dma")

    def __init__(self, eng, fn, epoch, dma):
        self.eng = eng
        self.fn = fn
        self.deps = []
        self.sig = False
        self.cnt = 0
        self.epoch = epoch
        self.dma = dma


class Sched:
    ENGS = ("pe", "act", "dve", "pool", "sp")

    def __init__(self):
        self.ops = {e: [] for e in self.ENGS}
        self.last_w = {}
        self.readers = {}
        self.epoch = 0
        self.dma_count = {}

    def op(self, eng, fn, reads=(), writes=(), dma=None, group=False):
        if dma is not None:
            self.dma_count[dma] = self.dma_count.get(dma, 0) + 16
            dma = (dma, None if group else self.dma_count[dma])
        o = Op(eng, fn, self.epoch, dma)
        deps = {}
        for r in reads:
            w = self.last_w.get(r)
            if w is not None:
                deps[id(w)] = w
        for r in writes:
            w = self.last_w.get(r)
            if w is not None:
                deps[id(w)] = w
            for rd in self.readers.get(r, ()):
                deps[id(rd)] = rd
        for d in deps.values():
            if d is o:
                continue
            if d.dma is None and d.eng == "pe" and eng == "pe":
                continue
            o.deps.append(d)
            if d.dma is None:
                d.sig = True
        for r in reads:
            self.readers.setdefault(r, []).append(o)
        for r in writes:
            self.last_w[r] = o
            self.readers[r] = []
        self.ops[eng].append(o)
        return o

    def emit(self, nc, block, engsem, final_waits):
        for e in self.ENGS:
            c = {}
            for o in self.ops[e]:
                if o.dma is None and o.sig:
                    c[o.epoch] = c.get(o.epoch, 0) + 1
                    o.cnt = c[o.epoch]

        def run(e, eng):
            known = {}
            for o in self.ops[e]:
                need = {}
                for d in o.deps:
                    if d.dma is not None:
                        s, v = d.dma
                        if v is None:
                            v = self.dma_count[s]
                    else:
                        s, v = engsem[d.eng][d.epoch], d.cnt
                    k = id(s)
                    if k not in need or need[k][1] < v:
                        need[k] = (s, v)
                for k, (s, v) in need.items():
                    if known.get(k, 0) >= v:
                        continue
                    eng.wait_ge(s, v)
                    known[k] = v
                ins = o.fn(eng)
                if o.dma is not None:
                    ins.then_inc(o.dma[0], 16)
                elif o.sig:
                    ins.then_inc(engsem[e][o.epoch], 1)
            if e == "sp":
                for s, v in final_waits:
                    eng.wait_ge(s, v)

        @block.tensor
        def _(eng):
            run("pe", eng)

        @block.scalar
        def _(eng):
            run("act", eng)

        @block.vector
        def _(eng):
            run("dve", eng)

        @block.gpsimd
        def _(eng):
            run("pool", eng)

        @block.sync
        def _(eng):
            run("sp", eng)


def MM(out, lhsT, rhs, start=True, stop=True):
    return lambda e: e.matmul(out, lhsT, rhs, start=start, stop=stop)


def TR(out, in_, ident):
    return lambda e: e.transpose(out, in_, ident)


def ACTF(out, in_, func, **kw):
    return lambda e: e.activation(out, in_, func, **kw)


def TT_(out, a, b, op):
    return lambda e: e.tensor_tensor(out, a, b, op)


def TS(out, a, s1, s2, op0, op1=None):
    if op1 is None:
        return lambda e: e.tensor_scalar(out, a, s1, None, op0)
    return lambda e: e.tensor_scalar(out, a, s1, s2, op0, op1)


def STT(out, a, s, b, op0, op1):
    return lambda e: e.scalar_tensor_tensor(out, a, s, b, op0, op1)


def CP(out, in_):
    return lambda e: e.tensor_copy(out, in_)


def MS(out, v):
    return lambda e: e.memset(out, v)


def RCP(out, in_):
    return lambda e: e.reciprocal(out, in_)


def DMA(out, in_):
    return lambda e: e.dma_start(out=out, in_=in_)


def build_nc(NSEQ, NT, stage=3):
    nc = bass.Bass("TRN2", target_bir_lowering=False)
    SEQL = NT * TT
    S = Sched()

    def din(name, shape, dt=F32):
        return nc.dram_tensor(name, list(shape), dt, kind="ExternalInput").ap()

    x = din("x", [NSEQ, SEQL, D])
    cin = din("c", [NSEQ, D])
    wsrc = {
        "gu1": din("w_gu1", [11, 128, 4096]),
        "d1": din("w_d1", [8, 128, 2816]),
        "in": din("w_in", [8, 128, 4096]),
        "out": din("w_out", [2, 128, 4096]),
        "gu2": din("w_gu2", [11, 128, 4096]),
        "d2": din("w_d2", [8, 128, 2816]),
    }
    wada = din("w_ada", [18, 128, 4096])
    wif_d = din("w_if", [128, 64])
    pvec_d = din("pvec", [128, PV_N])
    rows_d = din("rows", [4, 128, 512])
    gb_d = din("gate_b", [128, 8])
    ident_d = din("ident", [128, 128])
    cm_d = din("cmats", [128, 5 * 128])
    mask_d = din("maskm", [128, 512])
    y = nc.dram_tensor("y", [NSEQ, SEQL, D], F32, kind="ExternalOutput").ap()
    dbg = None
    dbgx = [0]
    dbgi = [0]
    wscr = {k: nc.dram_tensor("scr_" + k, list(v.shape), BF16, kind="Internal").ap()
            for k, v in wsrc.items()}

    es = ExitStack()
    with es:
        def sb(name, shape, dt=F32):
            return es.enter_context(nc.sbuf_tensor("sb_" + name, list(shape), dt))

        def sem(name):
            return es.enter_context(nc.semaphore("sem_" + name))

        ident = sb("ident", [128, 128])
        identb = sb("identb", [128, 128], BF16)
        onesb = sb("onesb", [128, 128], BF16)
        cm = sb("cm", [128, 5 * 128])
        TRIinc, ONESF, MID, TRIB, UPB = [cm[:, i * 128:(i + 1) * 128] for i in range(5)]
        maskm = sb("maskm", [128, 512])
        pvec = sb("pvec", [128, PV_N])
        gbb = sb("gbb", [128, 8])
        mgh = sb("mgh", [128, 512])
        hgb = sb("hgb", [128, 512])
        lbb = sb("lbb", [128, 512])
        omlh = sb("omlh", [128, 512])
        oml = sb("oml", [128, 512])
        wif = sb("wif", [128, 64], BF16)
        csb = sb("csb", [NSEQ, D])
        scT = sb("scT", [128, 8, NSEQ], BF16)
        modT = sb("modT", [128, 72, NSEQ])
        Amod = [sb("A%d" % i, [128, NSEQ, 8]) for i in range(3)]
        Bmod = [sb("B%d" % i, [128, NSEQ, 8]) for i in range(3)]
        Gmod = [sb("G%d" % i, [128, NSEQ, 8]) for i in range(3)]
        pgs = sb("pgs", [128, 48])
        xin = [sb("xin%d" % i, [128, D]) for i in range(4)]
        xo = [sb("xo%d" % i, [128, D]) for i in range(2)]
        hT = sb("hT", [128, 8, TT])
        yT = sb("yT", [128, 8, TT], BF16)
        U = sb("U", [128, 24, TT], BF16)
        oT = sb("oT", [128, 8, TT])
        sq = [sb("sq%d" % i, [128, TT], BF16) for i in range(2)]
        sg = [sb("sg%d" % i, [128, TT]) for i in range(2)]
        tmp = [sb("tmp%d" % i, [128, TT]) for i in range(2)]
        rstd = sb("rstd", [128, TT])
        ring = [sb("ring%d" % i, [128, SLOTLEN], BF16) for i in range(NSLOT)]
        qkpre = [sb("qkpre%d" % i, [128, TT + 3]) for i in range(2)]
        cacc = sb("cacc", [128, TT])
        halo = sb("halo", [128, 8, 3])
        qkT = sb("qkT", [128, 8, TT], BF16)
        gsb = sb("gsb", [128, 4, 8])
        sm = sb("sm", [128, 64])
        E = [sb("E%d" % i, [128, TT]) for i in range(2)]
        qt = sb("qt", [128, 512], BF16)
        kt = sb("kt", [128, 512], BF16)
        qb = sb("qb", [128, 512], BF16)
        kb = sb("kb", [128, 512], BF16)
        qtT = sb("qtT", [128, 512], BF16)
        ktTA = sb("ktTA", [128, 4, 128], BF16)
        ktTB = sb("ktTB", [128, 4, 128], BF16)
        qbTA = sb("qbTA", [128, 4, 128], BF16)
        qbTB = sb("qbTB", [128, 4, 128], BF16)
        attm = sb("attm", [128, 512], BF16)
        PT = sb("PT", [128, 512], BF16)
        ktok = sb("ktok", [128, 512], BF16)
        vb = sb("vb", [128, 4, 130], BF16)
        vg = sb("vg", [128, 4, 130], BF16)
        Cf = sb("Cf", [128, 4, 130])
        Cb = sb("Cb", [128, 4, 130], BF16)
        Sf = sb("Sf", [128, 4, 128])
        Sb = [sb("Sb%d" % i, [128, 4, 128], BF16) for i in range(2)]
        merged = sb("merged", [128, D], BF16)
        junk = sb("junk", [128, 128])
        ccol = sb("ccol", [128, 4])

        ps = [es.enter_context(nc.psum_tensor("psum%d" % i, [128, 512], F32)) for i in range(8)]

        def psb(i):
            return ps[i][:, :].bitcast(BF16)

        def Urow(r):
            return U[:, r, :]
        aT = lambda f: U[:, f, :]
        vtok = lambda c: U[:, 0 + c, :]
        gmo = lambda c: U[:, 4 + c, :]
        hqs = lambda c: U[:, 8 + c, :]
        kk = lambda c: U[:, 12 + c, :]
        vh = lambda c: U[:, 16 + c, :]
        gh = lambda c: U[:, 20 + c, :]
        lf = lambda c: oT[:, c, :]

        NEPOCH = NSEQ + 1
        engsem = {e: [sem("s_%s_%d" % (e, i)) for i in range(NEPOCH)]
                  for e in ("pe", "act", "dve", "pool")}
        engsem["sp"] = [None] * NEPOCH
        ring_sem = [sem("ring%d" % i) for i in range(NSLOT)]
        xin_sem = [sem("xin%d" % i) for i in range(4)]
        xo_sem = [sem("xo%d" % i) for i in range(2)]
        cast_sem = [sem("cast%d" % i) for i in range(4)]
        dsem = sem("dsem")
        csem = sem("csem")

        def cload(dst, src, w, q="sp"):
            S.op(q, DMA(dst, src), writes=w, dma=csem, group=GROUP_C)

        cload(ident[:, :], ident_d, ["ident"])
        cload(cm[:, :], cm_d, ["cm"])
        cload(maskm[:, :], mask_d, ["maskm"])
        cload(pvec[:, :], pvec_d, ["pvec"])
        cload(csb[:, :], cin, ["csb"])
        for r, (dst, tk) in enumerate(((mgh, "mgh"), (hgb, "hgb"), (lbb, "lbb"), (oml, "oml"))):
            cload(dst[:, :], rows_d[r], [tk])
        cload(gbb[:, :], gb_d, ["gbb"])
        S.op("pool", DMA(wif[:, :], wif_d), writes=["wif"], dma=csem, group=GROUP_C)

        S.op("dve", CP(identb[:, :], ident[:, :]), reads=["ident"], writes=["identb"])
        S.op("pool", MS(onesb[:, :], 1.0), writes=["onesb"])
        S.op("dve", MS(ccol[:, 0:1], D * EPS), writes=["ccol"])
        S.op("dve", TS(gbb[:, 0:4], gbb[:, 0:4], LNSCALE, None, ALU.add), reads=["gbb"], writes=["gbb"])
        S.op("dve", TS(mgh[:, :], mgh[:, :], 0.5, None, ALU.mult), reads=["mgh"], writes=["mgh"])
        S.op("dve", TT_(tmp[0][:, :], oml[:, :], lbb[:, :], ALU.subtract), reads=["oml", "lbb"], writes=[("tmp", 0)])
        S.op("act", ACTF(tmp[1][:, :], tmp[0][:, :], AF.Exp), reads=[("tmp", 0)], writes=[("tmp", 1)])
        S.op("dve", TS(tmp[1][:, :], tmp[1][:, :], 1.0, None, ALU.add), reads=[("tmp", 1)], writes=[("tmp", 1)])
        S.op("dve", RCP(lbb[:, :], tmp[1][:, :]), reads=[("tmp", 1)], writes=["lbb"])
        S.op("dve", TS(oml[:, :], lbb[:, :], -1.0, 1.0, ALU.mult, ALU.add), reads=["lbb"], writes=["oml"])
        S.op("dve", TS(omlh[:, :], oml[:, :], 0.5, None, ALU.mult), reads=["oml"], writes=["omlh"])
        for i in range(3):
            S.op("dve", TS(pgs[:, i * 8:(i + 1) * 8], pvec[:, PV_PRE[i]:PV_PRE[i] + 8], SQD, None, ALU.mult),
                 reads=["pvec"], writes=["pgs"])
            wgt = (0.5, 1.0, 0.5)[i]
            S.op("dve", TS(pgs[:, 24 + i * 8:24 + (i + 1) * 8], pvec[:, PV_POST[i]:PV_POST[i] + 8], SQD * wgt, None, ALU.mult),
                 reads=["pvec"], writes=["pgs"])

        S.op("act", ACTF(csb[:, :], csb[:, :], AF.Silu), reads=["csb"], writes=["csb"])
        for k in range(8):
            S.op("pe", TR(ps[7][:, k * NSEQ:(k + 1) * NSEQ], csb[:, k * 128:(k + 1) * 128], ident[0:NSEQ, 0:NSEQ]),
                 reads=["csb", "ident"], writes=[("ps", 7)])
        S.op("act", ACTF(scT[:, :, :].rearrange("p k b -> p (k b)"), ps[7][:, 0:8 * NSEQ], AF.Copy),
             reads=[("ps", 7)], writes=["scT"])

        class Ring:
            def __init__(self):
                self.reqs = []
                self.issued = 0

            def add(self, kind, j, length):
                self.reqs.append((kind, j, length))

            def ensure(self, upto):
                while self.issued < min(upto + 1, len(self.reqs)):
                    n = self.issued
                    kind, j, length = self.reqs[n]
                    s = n % NSLOT
                    if kind == "ada":
                        S.op("pool", DMA(ring[s][:, 0:length], wada[j]), writes=[("ws", s)], dma=ring_sem[s])
                    else:
                        S.op("sp", DMA(ring[s][:, 0:length], wscr[kind][j]), reads=[("scr", kind, j)],
                             writes=[("ws", s)], dma=ring_sem[s])
                    self.issued += 1

            def use(self, n):
                self.ensure(n + NSLOT - 1)
                s = n % NSLOT
                return ring[s], ("ws", s)

        R = Ring()
        for j in range(18):
            R.add("ada", j, 4096)
        tile_blocks = ([("gu1", j, 4096) for j in range(11)] + [("d1", j, 2816) for j in range(8)] +
                       [("in", j, 4096) for j in range(8)] + [("out", j, 4096) for j in range(2)] +
                       [("gu2", j, 4096) for j in range(11)] + [("d2", j, 2816) for j in range(8)])
        for _ in range(NSEQ * NT):
            for b_ in tile_blocks:
                R.add(*b_)
        rn = [0]

        def next_block():
            n = rn[0]
            rn[0] += 1
            return R.use(n)

        ci = 0
        for kind in ("gu1", "d1", "in", "out", "gu2", "d2"):
            for j in range(wsrc[kind].shape[0]):
                S.op("pool", DMA(wscr[kind][j], wsrc[kind][j]), writes=[("scr", kind, j), ("castslot", ci % 4)],
                     dma=cast_sem[ci % 4])
                ci += 1

        for jb in range(18):
            slot, tok = next_block()
            for fc in range(4):
                ch = jb * 4 + fc
                for k in range(8):
                    S.op("pe", MM(ps[6][:, ch * NSEQ:(ch + 1) * NSEQ],
                                  slot[:, k * 512 + fc * 128:k * 512 + (fc + 1) * 128], scT[:, k, :],
                                  start=(k == 0), stop=(k == 7)),
                         reads=[tok, "scT"], writes=[("ps", 6)])
        for b in range(NSEQ):
            S.op("dve", TT_(modT[:, :, b], ps[6][:, 0:72 * NSEQ].rearrange("p (c b) -> p c b", b=NSEQ)[:, :, b],
                            pvec[:, PV_ADAB:PV_ADAB + 72], ALU.add),
                 reads=[("ps", 6), "pvec"], writes=["modT"])
            for i in range(3):
                sh = modT[:, (3 * i) * 8:(3 * i) * 8 + 8, b]
                sc = modT[:, (3 * i + 1) * 8:(3 * i + 1) * 8 + 8, b]
                gg = modT[:, (3 * i + 2) * 8:(3 * i + 2) * 8 + 8, b]
                S.op("dve", STT(Amod[i][:, b, :], sc, 1.0, pgs[:, i * 8:(i + 1) * 8], ALU.add, ALU.mult),
                     reads=["modT", "pgs"], writes=["AB"])
                S.op("dve", CP(Bmod[i][:, b, :], sh), reads=["modT"], writes=["AB"])
                S.op("dve", TT_(Gmod[i][:, b, :], gg, pgs[:, 24 + i * 8:24 + (i + 1) * 8], ALU.mult),
                     reads=["modT", "pgs"], writes=["AB"])

        for t_ in (ktTA, ktTB, qbTA, qbTB):
            S.op("pool", MS(t_[:, :, :], 0.0), writes=["ktTA", "ktTB", "qbTA", "qbTB"])

        def sumsq_rstd(src_k, src_tok, use_pool):
            for k in range(8):
                sqb = sq[k % 2]
                if use_pool:
                    S.op("pool", TT_(sqb[:, :], src_k(k), src_k(k), ALU.mult), reads=[src_tok(k)], writes=[("sq", k % 2)])
                else:
                    S.op("act", ACTF(sqb[:, :], src_k(k), AF.Square), reads=[src_tok(k)], writes=[("sq", k % 2)])
                S.op("pe", MM(ps[6][:, :], onesb[:, :], sqb[:, :], start=(k == 0), stop=(k == 7)),
                     reads=[("sq", k % 2), "onesb"], writes=[("ps", 6)])
            S.op("act", ACTF(rstd[:, :], ps[6][:, :], AF.Ln, bias=ccol[:, 0:1]), reads=[("ps", 6), "ccol"], writes=["rstd"])
            S.op("act", ACTF(rstd[:, :], rstd[:, :], AF.Exp, scale=-0.5), reads=["rstd"], writes=["rstd"])

        def norm_mod(i, b):
            sumsq_rstd(lambda k: hT[:, k, :], lambda k: ("hT", k), False)
            for k in range(8):
                S.op("dve", TT_(tmp[k % 2][:, :], hT[:, k, :], rstd[:, :], ALU.mult),
                     reads=[("hT", k), "rstd"], writes=[("tmp", k % 2)])
                S.op("act", ACTF(yT[:, k, :], tmp[k % 2][:, :], AF.Identity,
                                 scale=Amod[i][:, b, k:k + 1], bias=Bmod[i][:, b, k:k + 1]),
                     reads=[("tmp", k % 2), "AB"], writes=[("yT", k)])

        def post_res(i, b):
            sumsq_rstd(lambda k: oT[:, k, :], lambda k: ("O", k), True)
            for k in range(8):
                S.op("dve", TT_(tmp[k % 2][:, :], oT[:, k, :], rstd[:, :], ALU.mult),
                     reads=[("O", k), "rstd"], writes=[("tmp", k % 2)])
                S.op("dve", STT(hT[:, k, :], tmp[k % 2][:, :], Gmod[i][:, b, k:k + 1], hT[:, k, :], ALU.mult, ALU.add),
                     reads=[("tmp", k % 2), "AB", ("hT", k)], writes=[("hT", k)])

        def ffn(i, b):
            norm_mod(i, b)
            yr = [("yT", k) for k in range(8)]
            for j in range(11):
                slot, tok = next_block()
                for fc in range(2):
                    f = j * 2 + fc
                    pg, pu = ps[f % 2], ps[2 + f % 2]
                    for k in range(8):
                        S.op("pe", MM(pg[:, :], slot[:, k * 256 + fc * 128:k * 256 + (fc + 1) * 128], yT[:, k, :],
                                      start=(k == 0), stop=(k == 7)), reads=[tok, ("yT", k)], writes=[("ps", f % 2)])
                    for k in range(8):
                        S.op("pe", MM(pu[:, :], slot[:, 2048 + k * 256 + fc * 128:2048 + k * 256 + (fc + 1) * 128], yT[:, k, :],
                                      start=(k == 0), stop=(k == 7)), reads=[tok, ("yT", k)], writes=[("ps", 2 + f % 2)])
                    S.op("act", ACTF(sg[f % 2][:, :], pg[:, :], AF.Silu), reads=[("ps", f % 2)], writes=[("sg", f % 2)])
                    S.op("dve", TT_(aT(f), sg[f % 2][:, :], pu[:, :], ALU.mult),
                         reads=[("sg", f % 2), ("ps", 2 + f % 2)], writes=[("U", f)])
            for dc in range(8):
                slot, tok = next_block()
                po = ps[4 + dc % 2]
                for f in range(NFC):
                    S.op("pe", MM(po[:, :], slot[:, f * 128:(f + 1) * 128], aT(f), start=(f == 0), stop=(f == NFC - 1)),
                         reads=[tok, ("U", f)], writes=[("ps", 4 + dc % 2)])
                S.op("act", ACTF(oT[:, dc, :], po[:, :], AF.Copy), reads=[("ps", 4 + dc % 2)], writes=[("O", dc)])
            post_res(i, b)

        def load_x(b, it):
            for c in range(4):
                t0 = it * TT + c * 128
                S.op("sp", DMA(xin[c][:, :], x[b, t0:t0 + 128, :]), writes=[("xin", c)], dma=xin_sem[c])

        def x_to_hT():
            for c in range(4):
                for kb_ in range(2):
                    bank = 4 + kb_
                    for kk_ in range(4):
                        k = kb_ * 4 + kk_
                        S.op("pe", TR(ps[bank][:, kk_ * 128:(kk_ + 1) * 128], xin[c][:, k * 128:(k + 1) * 128], ident[:, :]),
                             reads=[("xin", c), "ident"], writes=[("ps", bank)])
                    eng = "act" if kb_ == 0 else "dve"
                    dst = hT[:, kb_ * 4:(kb_ + 1) * 4, c * 128:(c + 1) * 128]
                    src = ps[bank][:, :].rearrange("p (k t) -> p k t", k=4)
                    fn = ACTF(dst, src, AF.Copy) if eng == "act" else CP(dst, src)
                    S.op(eng, fn, reads=[("ps", bank)], writes=[("hT", kb_ * 4 + q) for q in range(4)])

        def hT_to_y(b, it):
            for c in range(4):
                buf = xo[c % 2]
                for kb_ in range(2):
                    bank = 4 + kb_
                    for kk_ in range(4):
                        k = kb_ * 4 + kk_
                        S.op("pe", TR(ps[bank][:, kk_ * 128:(kk_ + 1) * 128], hT[:, k, c * 128:(c + 1) * 128], ident[:, :]),
                             reads=[("hT", k), "ident"], writes=[("ps", bank)])
                    eng = "act" if kb_ == 0 else "dve"
                    dst = buf[:, kb_ * 512:(kb_ + 1) * 512]
                    fn = ACTF(dst, ps[bank][:, :], AF.Copy) if eng == "act" else CP(dst, ps[bank][:, :])
                    S.op(eng, fn, reads=[("ps", bank)], writes=[("xo", c % 2, kb_)])
                t0 = it * TT + c * 128
                S.op("pool", DMA(y[b, t0:t0 + 128, :], buf[:, :]), reads=[("xo", c % 2, 0), ("xo", c % 2, 1)],
                     writes=[("xo", c % 2, 0), ("xo", c % 2, 1)], dma=xo_sem[c % 2])

        def mixer(b, first_tile):
            norm_mod(1, b)
            if first_tile:
                S.op("pool", MS(halo[:, :, :], 0.0), writes=["halo"])
                S.op("pool", MS(Cf[:, :, :], 0.0), writes=["Cf"])
                S.op("pool", MS(Cb[:, :, :], 0.0), writes=["Cb"])
                S.op("pool", MS(Sf[:, :, :], 0.0), writes=["Sf"])
                S.op("pool", MS(Sb[0][:, :, :], 0.0), writes=[("Sb", 0)])
            for blk in range(2):
                slot, tok = next_block()
                for fc in range(4):
                    ch = blk * 4 + fc
                    pq = ps[ch % 2]
                    qp = qkpre[ch % 2]
                    for k in range(8):
                        S.op("pe", MM(pq[:, :], slot[:, k * 512 + fc * 128:k * 512 + (fc + 1) * 128], yT[:, k, :],
                                      start=(k == 0), stop=(k == 7)), reads=[tok, ("yT", k)], writes=[("ps", ch % 2)])
                    S.op("act", ACTF(qp[:, 3:TT + 3], pq[:, :], AF.Copy), reads=[("ps", ch % 2)], writes=[("qkpre", ch % 2)])
                    S.op("pool", CP(qp[:, 0:3], halo[:, ch, :]), reads=["halo"], writes=[("qkpre", ch % 2, "h")])
                    rd = [("qkpre", ch % 2), ("qkpre", ch % 2, "h"), "pvec"]
                    S.op("dve", TS(cacc[:, :], qp[:, 0:TT], pvec[:, PV_CW + ch:PV_CW + ch + 1],
                                   pvec[:, PV_CB + ch:PV_CB + ch + 1], ALU.mult, ALU.add), reads=rd, writes=["cacc"])
                    for j in range(1, 4):
                        S.op("dve", STT(cacc[:, :], qp[:, j:TT + j], pvec[:, PV_CW + j * 8 + ch:PV_CW + j * 8 + ch + 1],
                                        cacc[:, :], ALU.mult, ALU.add), reads=rd + ["cacc"], writes=["cacc"])
                    S.op("pool", CP(halo[:, ch, :], qp[:, TT:TT + 3]), reads=[("qkpre", ch % 2)], writes=["halo"])
                    S.op("act", ACTF(qkT[:, ch, :], cacc[:, :], AF.Silu), reads=["cacc"], writes=[("qkT", ch)])
            for blk in range(2, 8):
                slot, tok = next_block()
                for c in range(4):
                    pp = ps[2 + c % 2]
                    pt = ("ps", 2 + c % 2)
                    for k in range(8):
                        S.op("pe", MM(pp[:, :], yT[:, k, c * 128:(c + 1) * 128], slot[:, k * 512:(k + 1) * 512],
                                      start=(k == 0), stop=(k == 7)), reads=[tok, ("yT", k)], writes=[pt])
                    t2 = tmp[c % 2]
                    tt2 = ("tmp", c % 2)
                    if blk == 2:
                        S.op("act", ACTF(vtok(c), pp[:, :], AF.Copy), reads=[pt], writes=[("U", 0 + c)])
                    elif blk == 3:
                        S.op("act", ACTF(t2[:, :], pp[:, :], AF.Tanh, scale=0.5), reads=[pt], writes=[tt2])
                        S.op("dve", STT(gmo(c), t2[:, :], 1.0, mgh[:, :], ALU.add, ALU.mult),
                             reads=[tt2, "mgh"], writes=[("U", 4 + c)])
                    elif blk == 4:
                        S.op("act", ACTF(hqs(c), pp[:, :], AF.Silu), reads=[pt], writes=[("U", 8 + c)])
                    elif blk == 5:
                        S.op("act", ACTF(t2[:, :], pp[:, :], AF.Tanh, scale=0.5), reads=[pt], writes=[tt2])
                        S.op("dve", STT(t2[:, :], t2[:, :], 1.0, omlh[:, :], ALU.add, ALU.mult),
                             reads=[tt2, "omlh"], writes=[tt2])
                        S.op("pool", TT_(kk(c), oml[:, :], t2[:, :], ALU.subtract), reads=[tt2, "oml"], writes=[("U", 12 + c)])
                        S.op("dve", TT_(lf(c), t2[:, :], lbb[:, :], ALU.add), reads=[tt2, "lbb"], writes=[("O", c)])
                    elif blk == 6:
                        S.op("act", ACTF(vh(c), pp[:, :], AF.Copy), reads=[pt], writes=[("U", 16 + c)])
                    else:
                        S.op("act", ACTF(t2[:, :], pp[:, :], AF.Silu), reads=[pt], writes=[tt2])
                        S.op("pool", TT_(gh(c), t2[:, :], hgb[:, :], ALU.mult), reads=[tt2, "hgb"], writes=[("U", 20 + c)])
            for c in range(4):
                for k in range(8):
                    S.op("pe", MM(ps[7][:, c * 8:(c + 1) * 8], yT[:, k, c * 128:(c + 1) * 128], wif[:, k * 8:(k + 1) * 8],
                                  start=(k == 0), stop=(k == 7)), reads=["wif", ("yT", k)], writes=[("ps", 7)])
            S.op("dve", TT_(gsb[:, :, :], ps[7][:, 0:32].rearrange("p (c g) -> p c g", g=8),
                            gbb[:, :].unsqueeze(1).to_broadcast([128, 4, 8]), ALU.add),
                 reads=[("ps", 7), "gbb"], writes=["gsb"])
            S.op("act", ACTF(oT[:, 0:4, :], oT[:, 0:4, :], AF.Ln), reads=[("O", c) for c in range(4)],
                 writes=[("O", c) for c in range(4)])
            for c in range(4):
                mlstm_chunk(c)
                hgrn_chunk(c)
                if dbg is not None:
                    S.op("pool", DMA(dbg[dbgi[0]], merged[:, :]), reads=["merged"], writes=[("dbgout", dbgi[0])], dma=dsem, group=True)
                    dbgi[0] += 1
                for k in range(8):
                    S.op("pe", TR(psb(6)[:, k * 128:(k + 1) * 128], merged[:, k * 128:(k + 1) * 128], identb[:, :]),
                         reads=["merged", "identb"], writes=[("ps", 6)])
                S.op("act", ACTF(yT[:, :, c * 128:(c + 1) * 128], psb(6).rearrange("p (k t) -> p k t", k=8), AF.Copy),
                     reads=[("ps", 6)], writes=[("yT", k) for k in range(8)])
            for half in range(2):
                slot, tok = next_block()
                for dcl in range(4):
                    dc = half * 4 + dcl
                    po = ps[4 + dc % 2]
                    for k in range(8):
                        S.op("pe", MM(po[:, :], slot[:, k * 512 + dcl * 128:k * 512 + (dcl + 1) * 128], yT[:, k, :],
                                      start=(k == 0), stop=(k == 7)), reads=[tok, ("yT", k)], writes=[("ps", 4 + dc % 2)])
                    S.op("act", ACTF(oT[:, dc, :], po[:, :], AF.Copy), reads=[("ps", 4 + dc % 2)], writes=[("O", dc)])
            post_res(1, b)

        def dump(ap, n, reads):
            if dbg is not None and dbgx[0] < NSEQ * NT * 4 + 16:
                S.op("pool", DMA(dbg[dbgx[0], :, 0:n], ap), reads=reads, writes=[("dbgout", dbgx[0])], dma=dsem, group=True)
                dbgx[0] += 1

        def mlstm_chunk(c):
            cols = slice(c * 128, (c + 1) * 128)
            li = gsb[:, c, 0:4]
            pf = gsb[:, c, 4:8]
            e1, sp_, efl, t1, beta, t2, gamma, dec, rr, ssq, t3 = [sm[:, i * 4:(i + 1) * 4] for i in range(11)]
            S.op("act", ACTF(e1, pf, AF.Exp, scale=-1.0), reads=["gsb"], writes=["m_e1"])
            S.op("act", ACTF(sp_, e1, AF.Ln, bias=1.0), reads=["m_e1"], writes=["m_sp"])
            S.op("pe", MM(ps[7][:, 32:36], TRIinc, sp_), reads=["m_sp", "cm"], writes=[("ps", 7)])
            S.op("pe", MM(ps[7][:, 36:40], ONESF, sp_), reads=["m_sp", "cm"], writes=[("ps", 7)])
            cum = ps[7][:, 32:36]
            tot = ps[7][:, 36:40]
            S.op("act", ACTF(efl, cum, AF.Exp), reads=[("ps", 7)], writes=["m_efl"])
            S.op("dve", TT_(t1, li, cum, ALU.add), reads=[("ps", 7), "gsb"], writes=["m_t1"])
            S.op("act", ACTF(beta, t1, AF.Exp), reads=["m_t1"], writes=["m_beta"])
            S.op("dve", TT_(t2, t1, tot, ALU.subtract), reads=[("ps", 7), "m_t1"], writes=["m_t2"])
            S.op("act", ACTF(gamma, t2, AF.Exp), reads=["m_t2"], writes=["m_gamma"])
            S.op("act", ACTF(dec, tot, AF.Exp, scale=-1.0), reads=[("ps", 7)], writes=["m_dec"])
            for h in range(4):
                S.op("pe", TR(psb(0)[:, h * 128:(h + 1) * 128], qkT[:, 4 + h, cols], identb[:, :]),
                     reads=[("qkT", 4 + h), "identb"], writes=[("ps", 0)])
            S.op("act", ACTF(ktok[:, :], psb(0)[:, 0:512], AF.Copy), reads=[("ps", 0)], writes=["ktok"])
            for h in range(4):
                S.op("act", ACTF(vb[:, h, 0:128], vtok(c)[:, h * 128:(h + 1) * 128], AF.Identity, scale=beta[:, h:h + 1]),
                     reads=[("U", c), "m_beta"], writes=["vb"])
                S.op("dve", TS(vg[:, h, 0:128], vtok(c)[:, h * 128:(h + 1) * 128], gamma[:, h:h + 1], None, ALU.mult),
                     reads=[("U", c), "m_gamma"], writes=["vg"])
            S.op("pool", CP(vb[:, :, 128], beta), reads=["m_beta"], writes=["vb"])
            S.op("pool", CP(vg[:, :, 128], gamma), reads=["m_gamma"], writes=["vg"])
            for h in range(4):
                S.op("pe", MM(ps[1][:, h * 128:(h + 1) * 128], qkT[:, 4 + h, cols], qkT[:, h, cols]),
                     reads=[("qkT", 4 + h), ("qkT", h)], writes=[("ps", 1)])
            S.op("dve", TT_(PT[:, :], ps[1][:, :], maskm[:, :], ALU.mult), reads=[("ps", 1), "maskm"], writes=["PT"])
            poh = lambda h: ps[2 + h // 2][:, (h % 2) * 130:(h % 2) * 130 + 129]
            for h in range(4):
                S.op("pe", MM(poh(h), PT[:, h * 128:(h + 1) * 128], vb[:, h, 0:129], start=True, stop=False),
                     reads=["PT", "vb"], writes=[("ps", 2 + h // 2)])
                S.op("pe", MM(poh(h), qkT[:, h, cols], Cb[:, h, 0:129], start=False, stop=True),
                     reads=[("qkT", h), "Cb"], writes=[("ps", 2 + h // 2)])
            for h in range(4):
                S.op("act", ACTF(rr[:, h:h + 1], poh(h)[:, 128:129], AF.Abs),
                     reads=[("ps", 2 + h // 2)], writes=["m_rr"])
            S.op("dve", TT_(rr, rr, efl, ALU.max), reads=["m_rr", "m_efl"], writes=["m_rr"])
            S.op("dve", RCP(rr, rr), reads=["m_rr"], writes=["m_rr"])
            S.op("pool", MS(ssq, 0.0), writes=["m_ssq"])
            for h in range(4):
                S.op("act", ACTF(junk[:, :], poh(h)[:, 0:128], AF.Square, accum_out=ssq[:, h:h + 1]),
                     reads=[("ps", 2 + h // 2), "m_ssq"], writes=["m_ssq", "junk"])
            S.op("dve", TT_(t3, rr, rr, ALU.mult), reads=["m_rr"], writes=["m_t3"])
            S.op("dve", TT_(t3, t3, ssq, ALU.mult), reads=["m_t3", "m_ssq"], writes=["m_t3"])
            S.op("dve", TS(t3, t3, 1.0 / 128.0, EPS, ALU.mult, ALU.add), reads=["m_t3"], writes=["m_t3"])
            S.op("act", ACTF(t3, t3, AF.Ln), reads=["m_t3"], writes=["m_t3"])
            S.op("act", ACTF(t3, t3, AF.Exp, scale=-0.5), reads=["m_t3"], writes=["m_t3"])
            S.op("dve", TT_(t3, t3, rr, ALU.mult), reads=["m_t3", "m_rr"], writes=["m_t3"])
            for h in range(4):
                S.op("dve", STT(merged[:, h * 128:(h + 1) * 128], poh(h)[:, 0:128], t3[:, h:h + 1],
                                gmo(c)[:, h * 128:(h + 1) * 128], ALU.mult, ALU.mult),
                     reads=[("ps", 2 + h // 2), "m_t3", ("U", 4 + c)], writes=["merged"])
            dump(qkT[:, 0, cols], 128, [("qkT", 0)])
            dump(qkT[:, 4, cols], 128, [("qkT", 4)])
            dump(sm[:, :], 64, ["m_e1", "m_sp", "m_efl", "m_t1", "m_beta", "m_t2", "m_gamma", "m_dec", "m_rr", "m_ssq", "m_t3"])
            dump(PT[:, 0:128], 128, ["PT"])
            dump(vb[:, 0, :], 130, ["vb"])
            dump(gsb[:, c, :], 8, ["gsb"])
            pch = lambda h: ps[4 + h // 2][:, (h % 2) * 130:(h % 2) * 130 + 129]
            for h in range(4):
                S.op("pe", MM(pch(h), ktok[:, h * 128:(h + 1) * 128], vg[:, h, 0:129]),
                     reads=["ktok", "vg"], writes=[("ps", 4 + h // 2)])
            for h in range(4):
                S.op("dve", STT(Cf[:, h, 0:129], Cf[:, h, 0:129], dec[:, h:h + 1], pch(h), ALU.mult, ALU.add),
                     reads=["Cf", "m_dec", ("ps", 4 + h // 2)], writes=["Cf"])
            S.op("act", ACTF(Cb[:, :, :], Cf[:, :, :], AF.Copy), reads=["Cf"], writes=["Cb"])

        def hgrn_chunk(c):
            lfc = lf(c)
            S.op("pe", MM(ps[0][:, :], MID, lfc), reads=[("O", c), "cm"], writes=[("ps", 0)])
            S.op("pe", MM(ps[1][:, :], TRIB, lfc), reads=[("O", c), "cm"], writes=[("ps", 1)])
            S.op("pe", MM(ps[2][:, :], UPB, lfc), reads=[("O", c), "cm"], writes=[("ps", 2)])
            for h in range(4):
                for X in range(2):
                    S.op("pe", MM(ps[7][:, 40 + h * 2 + X:41 + h * 2 + X],
                                  lfc[X * 64:(X + 1) * 64, h * 128:(h + 1) * 128],
                                  ONESF[X * 64:(X + 1) * 64, 0:1]),
                         reads=[("O", c), "cm"], writes=[("ps", 7)])
            hdec = sm[:, 48:56]
            S.op("act", ACTF(hdec, ps[7][:, 40:48], AF.Exp), reads=[("ps", 7)], writes=["h_dec"])
            S.op("act", ACTF(E[0][:, :], ps[0][:, :], AF.Exp), reads=[("ps", 0)], writes=[("E", 0)])
            S.op("dve", TT_(qt[:, :], hqs(c), E[0][:, :], ALU.mult), reads=[("E", 0), ("U", 8 + c)], writes=["qt"])
            S.op("act", ACTF(E[1][:, :], ps[0][:, :], AF.Exp, scale=-1.0), reads=[("ps", 0)], writes=[("E", 1)])
            S.op("pool", TT_(kt[:, :], kk(c), E[1][:, :], ALU.mult), reads=[("E", 1), ("U", 12 + c)], writes=["kt"])
            S.op("act", ACTF(E[0][:, :], ps[1][:, :], AF.Exp), reads=[("ps", 1)], writes=[("E", 0)])
            S.op("dve", TT_(qb[:, :], hqs(c), E[0][:, :], ALU.mult), reads=[("E", 0), ("U", 8 + c)], writes=["qb"])
            S.op("act", ACTF(E[1][:, :], ps[2][:, :], AF.Exp), reads=[("ps", 2)], writes=[("E", 1)])
            S.op("pool", TT_(kb[:, :], kk(c), E[1][:, :], ALU.mult), reads=[("E", 1), ("U", 12 + c)], writes=["kb"])
            for h in range(4):
                S.op("pe", TR(psb(3)[:, h * 128:(h + 1) * 128], qt[:, h * 128:(h + 1) * 128], identb[:, :]),
                     reads=["qt", "identb"], writes=[("ps", 3)])
            for h in range(4):
                S.op("pe", TR(psb(4)[:, h * 128:(h + 1) * 128], kt[:, h * 128:(h + 1) * 128], identb[:, :]),
                     reads=["kt", "identb"], writes=[("ps", 4)])
            for h in range(4):
                S.op("pe", TR(psb(5)[:, h * 128:(h + 1) * 128], qb[:, h * 128:(h + 1) * 128], identb[:, :]),
                     reads=["qb", "identb"], writes=[("ps", 5)])
            S.op("act", ACTF(qtT[:, :], psb(3)[:, 0:512], AF.Copy), reads=[("ps", 3)], writes=["qtT"])
            v4 = psb(4)[:, 0:512].rearrange("p (h t) -> p h t", h=4)
            v5 = psb(5)[:, 0:512].rearrange("p (h t) -> p h t", h=4)
            S.op("dve", CP(ktTA[:, :, 0:64], v4[:, :, 0:64]), reads=[("ps", 4)], writes=["ktTA"])
            S.op("dve", CP(ktTB[:, :, 64:128], v4[:, :, 64:128]), reads=[("ps", 4)], writes=["ktTB"])
            S.op("act", ACTF(qbTA[:, :, 0:64], v5[:, :, 0:64], AF.Copy), reads=[("ps", 5)], writes=["qbTA"])
            S.op("act", ACTF(qbTB[:, :, 64:128], v5[:, :, 64:128], AF.Copy), reads=[("ps", 5)], writes=["qbTB"])
            for h in range(4):
                S.op("pe", MM(ps[6][:, h * 128:h * 128 + 64], ktTA[:, h, :], qtT[:, h * 128:h * 128 + 64]),
                     reads=["ktTA", "qtT"], writes=[("ps", 6)])
                S.op("pe", MM(ps[6][:, h * 128 + 64:h * 128 + 128], ktTB[:, h, :], qtT[:, h * 128 + 64:h * 128 + 128]),
                     reads=["ktTB", "qtT"], writes=[("ps", 6)])
            S.op("dve", TT_(attm[:, :], ps[6][:, :], maskm[:, :], ALU.mult), reads=[("ps", 6), "maskm"], writes=["attm"])
            for X in range(2):
                for h in range(4):
                    poh = ps[0][:, h * 128:(h + 1) * 128]
                    if X == 0:
                        S.op("pe", MM(poh, attm[:, h * 128:(h + 1) * 128], vh(c)[:, h * 128:(h + 1) * 128],
                                      start=(h == 0), stop=False), reads=["attm", ("U", 16 + c)], writes=[("ps", 0)])
                        S.op("pe", MM(poh, qbTA[:, h, :], Sb[0][:, h, :], start=False, stop=False),
                             reads=["qbTA", ("Sb", 0)], writes=[("ps", 0)])
                    else:
                        S.op("pe", MM(poh, qbTB[:, h, :], Sb[1][:, h, :], start=False, stop=True),
                             reads=["qbTB", ("Sb", 1)], writes=[("ps", 0)])
                    S.op("pe", MM(ps[1][:, h * 128:(h + 1) * 128], kb[X * 64:(X + 1) * 64, h * 128:(h + 1) * 128],
                                  vh(c)[X * 64:(X + 1) * 64, h * 128:(h + 1) * 128]),
                         reads=["kb", ("U", 16 + c)], writes=[("ps", 1)])
                for h in range(4):
                    S.op("dve", STT(Sf[:, h, :], Sf[:, h, :], hdec[:, h * 2 + X:h * 2 + X + 1],
                                    ps[1][:, h * 128:(h + 1) * 128], ALU.mult, ALU.add),
                         reads=["Sf", "h_dec", ("ps", 1)], writes=["Sf"])
                S.op("act", ACTF(Sb[(X + 1) % 2][:, :, :], Sf[:, :, :], AF.Copy), reads=["Sf"], writes=[("Sb", (X + 1) % 2)])
            ssq2 = sm[:, 56:60]
            rs2 = sm[:, 60:64]
            S.op("pool", MS(ssq2, 0.0), writes=["h_ssq"])
            for h in range(4):
                S.op("act", ACTF(junk[:, :], ps[0][:, h * 128:(h + 1) * 128], AF.Square, accum_out=ssq2[:, h:h + 1]),
                     reads=[("ps", 0), "h_ssq"], writes=["h_ssq", "junk"])
            S.op("dve", TS(rs2, ssq2, 1.0 / 128.0, EPS, ALU.mult, ALU.add), reads=["h_ssq"], writes=["h_rs"])
            S.op("act", ACTF(rs2, rs2, AF.Ln), reads=["h_rs"], writes=["h_rs"])
            S.op("act", ACTF(rs2, rs2, AF.Exp, scale=-0.5), reads=["h_rs"], writes=["h_rs"])
            for h in range(4):
                S.op("dve", STT(merged[:, 512 + h * 128:512 + (h + 1) * 128], ps[0][:, h * 128:(h + 1) * 128],
                                rs2[:, h:h + 1], gh(c)[:, h * 128:(h + 1) * 128], ALU.mult, ALU.mult),
                     reads=[("ps", 0), "h_rs", ("U", 20 + c)], writes=["merged"])

        load_x(0, 0)
        tiles = [(b, it) for b in range(NSEQ) for it in range(NT)]
        for n, (b, it) in enumerate(tiles):
            S.epoch = b + 1
            x_to_hT()
            if n + 1 < len(tiles):
                load_x(*tiles[n + 1])
            if stage >= 1:
                ffn(0, b)
            else:
                for _ in range(19):
                    next_block()
            if stage >= 2:
                mixer(b, it == 0)
            else:
                for _ in range(10):
                    next_block()
            if stage >= 3:
                ffn(2, b)
            else:
                for _ in range(19):
                    next_block()
            hT_to_y(b, it)

        final_waits = [(xo_sem[i], S.dma_count.get(xo_sem[i], 0)) for i in range(2)]
        if S.dma_count.get(dsem, 0):
            final_waits.append((dsem, S.dma_count[dsem]))
        with nc.Block() as block:
            S.emit(nc, block, engsem, final_waits)
    return nc


def _fm(v):
    return np.ascontiguousarray(np.asarray(v, np.float32).reshape(8, 128).T)


def _prep_weights(inp):
    f32 = np.float32

    def gu(wg, wu):
        g = np.asarray(wg[0], f32).reshape(8, 128, 11, 256).transpose(2, 1, 0, 3)
        u = np.asarray(wu[0], f32).reshape(8, 128, 11, 256).transpose(2, 1, 0, 3)
        return np.ascontiguousarray(np.stack([g, u], axis=2).reshape(11, 128, 4096))

    def dn(wd):
        return np.ascontiguousarray(np.asarray(wd[0], f32).reshape(22, 128, 8, 128).transpose(2, 1, 0, 3).reshape(8, 128, 2816))

    w_in = np.asarray(inp["w_in"][0], f32)
    main = np.concatenate([w_in[:, 0:2048], w_in[:, 2056:4104]], axis=1)
    win = np.ascontiguousarray(main.reshape(8, 128, 8, 512).transpose(2, 1, 0, 3).reshape(8, 128, 4096))
    wif = np.ascontiguousarray(w_in[:, 2048:2056].reshape(8, 128, 8).transpose(1, 0, 2).reshape(128, 64))
    wout = np.ascontiguousarray(np.asarray(inp["w_out"][0], f32).reshape(8, 128, 2, 512).transpose(2, 1, 0, 3).reshape(2, 128, 4096))
    wada = np.ascontiguousarray(np.asarray(inp["ada_w"][0], f32).reshape(8, 128, 18, 512).transpose(2, 1, 0, 3).reshape(18, 128, 4096))
    pvec = np.zeros((128, PV_N), f32)
    for i, nm in enumerate(("ffn1_pre_g", "mix_pre_g", "ffn2_pre_g")):
        pvec[:, PV_PRE[i]:PV_PRE[i] + 8] = _fm(inp[nm][0])
    for i, nm in enumerate(("ffn1_post_g", "mix_post_g", "ffn2_post_g")):
        pvec[:, PV_POST[i]:PV_POST[i] + 8] = _fm(inp[nm][0])
    pvec[:, PV_ADAB:PV_ADAB + 72] = np.asarray(inp["ada_b"][0], f32).reshape(72, 128).T
    cw = np.asarray(inp["mlstm_conv_w"][0], f32)
    for j in range(4):
        pvec[:, PV_CW + j * 8:PV_CW + j * 8 + 8] = _fm(cw[j])
    pvec[:, PV_CB:PV_CB + 8] = _fm(inp["mlstm_conv_b"][0])
    rows = np.stack([np.asarray(inp["mlstm_norm_g"][0], f32), np.asarray(inp["hgrn_norm_g"][0], f32),
                     np.asarray(inp["hgrn_lb_logits"][0], f32), np.asarray(inp["hgrn_lb_logits"][1], f32)])
    rows = np.ascontiguousarray(np.broadcast_to(rows[:, None, :], (4, 128, 512)))
    s = np.arange(128)[:, None]
    t = np.arange(128)[None, :]
    same = (s // 64) == (t // 64)
    r = (t // 64) * 64 + 31
    TRIinc = (s <= t).astype(f32)
    ONES = np.ones((128, 128), f32)
    MIDm = (same & (s > r) & (s <= t)).astype(f32) - (same & (s > t) & (s <= r)).astype(f32)
    TRIBm = (same & (s <= t)).astype(f32)
    UPBm = (same & (s > t)).astype(f32)
    cm = np.ascontiguousarray(np.concatenate([TRIinc, ONES, MIDm, TRIBm, UPBm], axis=1))
    maskm = np.ascontiguousarray(np.tile(TRIinc, (1, 4)))
    return {
        "w_gu1": gu(inp["ffn1_w_gate"], inp["ffn1_w_up"]), "w_d1": dn(inp["ffn1_w_down"]),
        "w_in": win, "w_out": wout,
        "w_gu2": gu(inp["ffn2_w_gate"], inp["ffn2_w_up"]), "w_d2": dn(inp["ffn2_w_down"]),
        "w_ada": wada, "w_if": wif, "pvec": pvec, "rows": rows,
        "gate_b": np.ascontiguousarray(np.broadcast_to(np.asarray(inp["mlstm_gate_b"], f32).reshape(1, 8), (128, 8))),
        "ident": np.eye(128, dtype=f32), "cmats": cm, "maskm": maskm,
    }


_NC_CACHE = {}


def run(inp, ncores, nseq, nt, stage=3):
    key = (nseq, nt, stage)
    if key not in _NC_CACHE:
        _NC_CACHE[key] = build_nc(nseq, nt, stage)
    nc = _NC_CACHE[key]
    shared = _prep_weights(inp)
    x = np.asarray(inp["x"], np.float32)
    c = np.asarray(inp["c"], np.float32)
    in_maps = []
    for i in range(ncores):
        m = dict(shared)
        m["x"] = np.ascontiguousarray(x[i * nseq:(i + 1) * nseq])
        m["c"] = np.ascontiguousarray(c[i * nseq:(i + 1) * nseq])
        in_maps.append(m)
    res = run_bass_kernel_spmd(nc, in_maps, core_ids=list(range(ncores)))
    return np.concatenate([np.asarray(r["y"]) for r in res.results], axis=0)


LAUNCH_CORES = int(os.environ.get("K_LC", "1"))


def kernel(**inputs):
    B, SL, _ = inputs["x"].shape
    nseq = B // NCORES
    if LAUNCH_CORES == NCORES:
        out = run(inputs, NCORES, nseq, SL // TT)
        return out.astype(np.float32, copy=False)
    outs = []
    per = nseq * LAUNCH_CORES
    for g in range(B // per):
        sub = dict(inputs)
        sub["x"] = inputs["x"][g * per:(g + 1) * per]
        sub["c"] = inputs["c"][g * per:(g + 1) * per]
        outs.append(run(sub, LAUNCH_CORES, nseq, SL // TT))
    return np.concatenate(outs, axis=0).astype(np.float32, copy=False)
```
